# Optimizing a Trainium2 kernel written in Bass

```python
import jax
import jax.numpy as jnp
from jax import lax
import numpy as np

D_MODEL = 1024
BATCH = 1
SEQ = 16384
DEPTH = 4

CHUNK = 64
PLE_DIM = 256
N_MIXERS = 3
LN_EPS = 1e-5
DEEPNORM_ALPHA = (2 * DEPTH) ** 0.25
DEEPNORM_BETA = (8 * DEPTH) ** -0.25
A_HEADS = 4
A_DK = D_MODEL // 8
A_DV = D_MODEL // 4
A_CONV = 4
B_HEADS = 4
B_DK = D_MODEL // 8
B_DV = D_MODEL // 4
B_RANK = 16
B_TAU = 16.0
C_EXPAND = 128
C_HEADS = D_MODEL // C_EXPAND
C_DV = D_MODEL // C_HEADS
N_EXPERTS = 16
N_GROUPS = 4
EXPERTS_PER_GROUP = N_EXPERTS // N_GROUPS
TOP_K = 2
D_EXPERT = D_MODEL // 2
N_A = (DEPTH + N_MIXERS - 1) // N_MIXERS
N_B = (DEPTH + N_MIXERS - 2) // N_MIXERS
N_C = DEPTH // N_MIXERS
A_PROJ = 2 * A_HEADS * A_DK + 2 * A_HEADS * A_DV + 2 * A_HEADS
B_PROJ = 2 * B_HEADS * B_DK + 2 * B_HEADS * B_DV + B_RANK
C_PROJ = 2 * C_HEADS * C_EXPAND + 2 * C_HEADS * C_DV

kernel_name = 'hybrid_mlstm_gla_hgrn2_moe_trunk'


def layer_norm(x, g, b):
    xf = x.astype(jnp.float32)
    mu = xf.mean(-1, keepdims=True)
    var = jnp.square(xf - mu).mean(-1, keepdims=True)
    y = (xf - mu) * lax.rsqrt(var + LN_EPS) * g.astype(jnp.float32) + b.astype(jnp.float32)
    return y.astype(x.dtype)


def head_rms_norm(h, g):
    bsz, seq, nh, e = h.shape
    hn = h * lax.rsqrt(jnp.mean(h * h, -1, keepdims=True) + LN_EPS)
    return hn.reshape(bsz, seq, nh * e) * g.astype(jnp.float32)


def causal_depthwise_conv(u, w):
    width, ch = w.shape
    return lax.conv_general_dilated(u, w[:, None, :].astype(u.dtype), window_strides=(1,),
                                    padding=[(width - 1, 0)],
                                    dimension_numbers=('NWC', 'WIO', 'NWC'),
                                    feature_group_count=ch)


def to_chunks(t):
    bsz, seq, nh, d = t.shape
    return t.reshape(bsz, seq // CHUNK, CHUNK, nh, d).transpose(1, 0, 3, 2, 4)


def from_chunks(t):
    nc, bsz, nh, l, d = t.shape
    return t.transpose(1, 0, 3, 2, 4).reshape(bsz, nc * l, nh, d)


def mlstm_chunkwise(q, k, v, log_i, log_f):
    bsz, _, nh, dk = q.shape
    dv = v.shape[-1]
    tri = jnp.tril(jnp.ones((CHUNK, CHUNK), dtype=bool))

    def step(carry, xs):
        c_st, n_st, m_st = carry
        qj, ks, vs, li, lf = xs
        a = jnp.cumsum(lf, axis=-1)
        a_tot = a[..., -1]
        d_log = jnp.where(tri, a[..., :, None] - a[..., None, :] + li[..., None, :], -jnp.inf)
        e_log = a + m_st[..., None]
        m_row = jnp.maximum(e_log, d_log.max(-1))
        w_intra = jnp.exp(d_log - m_row[..., None])
        w_inter = jnp.exp(e_log - m_row)
        qk = jnp.einsum('bhjd,bhsd->bhjs', qj, ks) * w_intra
        num = (w_inter[..., None] * jnp.einsum('bhjd,bhde->bhje', qj, c_st)
               + jnp.einsum('bhjs,bhse->bhje', qk, vs))
        den = w_inter * jnp.einsum('bhjd,bhd->bhj', qj, n_st) + qk.sum(-1)
        h = num / jnp.maximum(jnp.abs(den), jnp.exp(-m_row))[..., None]
        g_log = a_tot[..., None] - a + li
        m_new = jnp.maximum(a_tot + m_st, g_log.max(-1))
        decay = jnp.exp(a_tot + m_st - m_new)
        wk = jnp.exp(g_log - m_new[..., None])[..., None] * ks
        c_st = decay[..., None, None] * c_st + jnp.einsum('bhsd,bhse->bhde', wk, vs)
        n_st = decay[..., None] * n_st + wk.sum(-2)
        return (c_st, n_st, m_new), h

    init = (jnp.zeros((bsz, nh, dk, dv), jnp.float32),
            jnp.zeros((bsz, nh, dk), jnp.float32),
            jnp.zeros((bsz, nh), jnp.float32))
    xs = (to_chunks(q), to_chunks(k), to_chunks(v),
          to_chunks(log_i[..., None])[..., 0], to_chunks(log_f[..., None])[..., 0])
    _, h = lax.scan(step, init, xs)
    return from_chunks(h)


def gated_linear_attention_chunkwise(q, k, v, log_a):
    bsz, _, nh, dk = q.shape
    dv = v.shape[-1]
    tri = jnp.tril(jnp.ones((CHUNK, CHUNK), dtype=bool))[:, :, None]

    def step(s_st, xs):
        qj, ks, vs, la = xs
        b = jnp.cumsum(la, axis=-2)
        b_last = b[..., -1:, :]
        inter = jnp.einsum('bhjd,bhde->bhje', qj * jnp.exp(b), s_st)
        pair = jnp.exp(jnp.where(tri, b[..., :, None, :] - b[..., None, :, :], -jnp.inf))
        att = jnp.einsum('bhjsd,bhsd->bhjs', qj[..., :, None, :] * pair, ks)
        o = inter + jnp.einsum('bhjs,bhse->bhje', att, vs)
        s_st = (jnp.exp(b_last[..., 0, :])[..., None] * s_st
                + jnp.einsum('bhsd,bhse->bhde', ks * jnp.exp(b_last - b), vs))
        return s_st, o

    init = jnp.zeros((bsz, nh, dk, dv), jnp.float32)
    _, o = lax.scan(step, init, (to_chunks(q), to_chunks(k), to_chunks(v), to_chunks(log_a)))
    return from_chunks(o)


def mlstm_mixer(x, w_in, conv_w, gate_b, norm_g, w_out):
    bsz, seq, _ = x.shape
    nqk, nv = A_HEADS * A_DK, A_HEADS * A_DV
    z = x @ w_in
    qk, v, o, gates = jnp.split(z, [2 * nqk, 2 * nqk + nv, 2 * nqk + 2 * nv], axis=-1)
    qk = jax.nn.silu(causal_depthwise_conv(qk, conv_w)).astype(jnp.float32)
    q = qk[..., :nqk].reshape(bsz, seq, A_HEADS, A_DK)
    k = qk[..., nqk:].reshape(bsz, seq, A_HEADS, A_DK) * (A_DK ** -0.5)
    v = v.astype(jnp.float32).reshape(bsz, seq, A_HEADS, A_DV)
    gates = gates.astype(jnp.float32) + gate_b.astype(jnp.float32)
    log_i = gates[..., :A_HEADS]
    log_f = jax.nn.log_sigmoid(gates[..., A_HEADS:])
    h = mlstm_chunkwise(q, k, v, log_i, log_f)
    y = head_rms_norm(h, norm_g) * jax.nn.sigmoid(o.astype(jnp.float32))
    return y.astype(x.dtype) @ w_out


def gla_mixer(x, w_in, w_a2, b_a, norm_g, w_out):
    bsz, seq, _ = x.shape
    nk, nv = B_HEADS * B_DK, B_HEADS * B_DV
    z = x @ w_in
    q, k, v, g, a_lr = jnp.split(z, [nk, 2 * nk, 2 * nk + nv, 2 * nk + 2 * nv], axis=-1)
    log_a = jax.nn.log_sigmoid((a_lr @ w_a2 + b_a).astype(jnp.float32)) / B_TAU
    shp = (bsz, seq, B_HEADS, B_DK)
    o = gated_linear_attention_chunkwise(q.astype(jnp.float32).reshape(shp) * (B_DK ** -0.5),
                                         k.astype(jnp.float32).reshape(shp),
                                         v.astype(jnp.float32).reshape(bsz, seq, B_HEADS, B_DV),
                                         log_a.reshape(shp))
    y = head_rms_norm(o, norm_g) * jax.nn.silu(g.astype(jnp.float32))
    return y.astype(x.dtype) @ w_out


def hgrn2_mixer(x, w_in, lower_bound, norm_g, w_out):
    bsz, seq, _ = x.shape
    nk = C_HEADS * C_EXPAND
    z = x @ w_in
    q, f, i, g = jnp.split(z, [nk, 2 * nk, 2 * nk + C_HEADS * C_DV], axis=-1)
    shp = (bsz, seq, C_HEADS, C_EXPAND)
    lb = lower_bound.reshape(C_HEADS, C_EXPAND)
    fg = lb + (1.0 - lb) * jax.nn.sigmoid(f.astype(jnp.float32).reshape(shp))
    o = gated_linear_attention_chunkwise(jax.nn.silu(q.astype(jnp.float32)).reshape(shp),
                                         1.0 - fg,
                                         i.astype(jnp.float32).reshape(bsz, seq, C_HEADS, C_DV),
                                         jnp.log(fg))
    y = head_rms_norm(o, norm_g) * jax.nn.sigmoid(g.astype(jnp.float32))
    return y.astype(x.dtype) @ w_out


def moe_channel_mixer(x, router_w, router_b, w_gate, w_up, w_down):
    bsz, seq, d = x.shape
    t = x.reshape(-1, d)
    scores = jax.nn.softmax((t @ router_w).astype(jnp.float32), axis=-1)
    sel = scores + router_b.astype(jnp.float32)
    grp_score = lax.top_k(sel.reshape(-1, N_GROUPS, EXPERTS_PER_GROUP), TOP_K)[0].sum(-1)
    g_idx = jnp.argmax(grp_score, axis=-1)
    in_group = (jnp.arange(N_EXPERTS) // EXPERTS_PER_GROUP)[None, :] == g_idx[:, None]
    _, e_idx = lax.top_k(jnp.where(in_group, sel, -jnp.inf), TOP_K)
    w = jnp.take_along_axis(scores, e_idx, axis=-1)
    w = w / w.sum(-1, keepdims=True)
    gate = (jax.nn.one_hot(e_idx, N_EXPERTS, dtype=jnp.float32) * w[..., None]).sum(1)
    y = jnp.zeros(t.shape, jnp.float32)
    for e in range(N_EXPERTS):
        h = jax.nn.silu(t @ w_gate[e]) * (t @ w_up[e])
        y = y + gate[:, e:e + 1] * (h @ w_down[e]).astype(jnp.float32)
    return y.astype(x.dtype).reshape(bsz, seq, d)


def setup_inputs(seed: int = 0) -> dict:
    key = jax.random.key(seed)
    ks = jax.random.split(key, 32)
    nrm = lambda k, shape, scale: scale * jax.random.normal(k, shape, jnp.float32)
    d = D_MODEL
    return {
        'x': nrm(ks[0], (BATCH, SEQ, d), 1.0),
        'p': nrm(ks[1], (DEPTH, BATCH, SEQ, PLE_DIM), 1.0),
        'ln1_g': 1.0 + nrm(ks[2], (DEPTH, d), 0.02),
        'ln1_b': nrm(ks[3], (DEPTH, d), 0.02),
        'ln2_g': 1.0 + nrm(ks[4], (DEPTH, d), 0.02),
        'ln2_b': nrm(ks[5], (DEPTH, d), 0.02),
        'mlstm_w_in': nrm(ks[6], (N_A, d, A_PROJ), d ** -0.5),
        'mlstm_conv': nrm(ks[7], (N_A, A_CONV, 2 * A_HEADS * A_DK), A_CONV ** -0.5),
        'mlstm_gate_b': jnp.concatenate([nrm(ks[8], (N_A, A_HEADS), 0.1),
                                         3.0 + nrm(ks[9], (N_A, A_HEADS), 0.5)], axis=-1),
        'mlstm_norm_g': 1.0 + nrm(ks[10], (N_A, A_HEADS * A_DV), 0.02),
        'mlstm_w_out': nrm(ks[11], (N_A, A_HEADS * A_DV, d), DEEPNORM_BETA * (A_HEADS * A_DV) ** -0.5),
        'gla_w_in': nrm(ks[12], (N_B, d, B_PROJ), d ** -0.5),
        'gla_w_a2': nrm(ks[13], (N_B, B_RANK, B_HEADS * B_DK), B_RANK ** -0.5),
        'gla_b_a': nrm(ks[14], (N_B, B_HEADS * B_DK), 0.1),
        'gla_norm_g': 1.0 + nrm(ks[15], (N_B, B_HEADS * B_DV), 0.02),
        'gla_w_out': nrm(ks[16], (N_B, B_HEADS * B_DV, d), DEEPNORM_BETA * (B_HEADS * B_DV) ** -0.5),
        'hgrn_w_in': nrm(ks[17], (N_C, d, C_PROJ), d ** -0.5),
        'hgrn_lb': nrm(ks[18], (DEPTH, C_HEADS * C_EXPAND), 0.5),
        'hgrn_norm_g': 1.0 + nrm(ks[19], (N_C, C_HEADS * C_DV), 0.02),
        'hgrn_w_out': nrm(ks[20], (N_C, C_HEADS * C_DV, d), DEEPNORM_BETA * (C_HEADS * C_DV) ** -0.5),
        'router_w': nrm(ks[21], (d, N_EXPERTS), d ** -0.5),
        'router_b': nrm(ks[22], (N_EXPERTS,), 0.01),
        'exp_w_gate': nrm(ks[23], (DEPTH, N_EXPERTS, d, D_EXPERT), d ** -0.5),
        'exp_w_up': nrm(ks[24], (DEPTH, N_EXPERTS, d, D_EXPERT), d ** -0.5),
        'exp_w_down': nrm(ks[25], (DEPTH, N_EXPERTS, D_EXPERT, d), DEEPNORM_BETA * D_EXPERT ** -0.5),
        'ple_w_proj': nrm(ks[26], (DEPTH, PLE_DIM, d), PLE_DIM ** -0.5),
        'ple_w_gate': nrm(ks[27], (DEPTH, d, d), d ** -0.5),
    }


def reference(x, p, ln1_g, ln1_b, ln2_g, ln2_b,
              mlstm_w_in, mlstm_conv, mlstm_gate_b, mlstm_norm_g, mlstm_w_out,
              gla_w_in, gla_w_a2, gla_b_a, gla_norm_g, gla_w_out,
              hgrn_w_in, hgrn_lb, hgrn_norm_g, hgrn_w_out,
              router_w, router_b, exp_w_gate, exp_w_up, exp_w_down,
              ple_w_proj, ple_w_gate):
    lbs = jax.nn.softmax(hgrn_lb.astype(jnp.float32), axis=0)
    lbs = jnp.cumsum(lbs, axis=0) - lbs[0]
    for i in range(DEPTH):
        kind, slot = i % N_MIXERS, i // N_MIXERS
        if kind == 0:
            mix = mlstm_mixer(x, mlstm_w_in[slot], mlstm_conv[slot], mlstm_gate_b[slot],
                              mlstm_norm_g[slot], mlstm_w_out[slot])
        elif kind == 1:
            mix = gla_mixer(x, gla_w_in[slot], gla_w_a2[slot], gla_b_a[slot],
                            gla_norm_g[slot], gla_w_out[slot])
        else:
            mix = hgrn2_mixer(x, hgrn_w_in[slot], lbs[i], hgrn_norm_g[slot], hgrn_w_out[slot])
        x = layer_norm(DEEPNORM_ALPHA * x + mix, ln1_g[i], ln1_b[i])
        ffn = moe_channel_mixer(x, router_w, router_b, exp_w_gate[i], exp_w_up[i], exp_w_down[i])
        x = layer_norm(DEEPNORM_ALPHA * x + ffn, ln2_g[i], ln2_b[i])
        x = x + jax.nn.sigmoid(x @ ple_w_gate[i]) * (p[i] @ ple_w_proj[i])
    return x
```

```python
import numpy as np
from contextlib import ExitStack
import concourse.bass as bass
import concourse.mybir as mybir
from concourse.bass_utils import run_bass_kernel_spmd

F32 = mybir.dt.float32
BF16 = mybir.dt.bfloat16
AF = mybir.ActivationFunctionType
ALU = mybir.AluOpType
AX = mybir.AxisListType

NCORES = 8
D = 1024
SEQ = 16384
T = SEQ // NCORES
NT = T // 128
TB = 512
NB = T // TB
TPB = TB // 128
DEPTH = 4
PLE = 256
NEXP = 16
DEXP = 512
LN_EPS = 1e-5
ALPHA = (2 * DEPTH) ** 0.25
KINDS = ["A", "B", "C", "A"]
BIG = 1.0e4

CFG = {
    "A": dict(H=4, DV=256, lmul=-1.0, gact=AF.Sigmoid),
    "B": dict(H=4, DV=256, lmul=-1.0 / 16.0, gact=AF.Silu),
    "C": dict(H=8, DV=128, lmul=1.0, gact=AF.Sigmoid),
}

SAME_ENGINE_SYNC = True


class _Sem:
    def __init__(self, h, name):
        self.h = h
        self.v = 0
        self.name = name


class Prog:
    ENGS = ("pe", "act", "dve", "pool", "sp")

    def __init__(self, nc, stack):
        self.nc = nc
        self.stack = stack
        self.q = {e: [] for e in self.ENGS}
        self.esem = {}
        self.nsem = 0
        self.allsems = []
        for e in ("pe", "act", "dve", "pool"):
            self.esem[e] = self.new_sem("c_" + e)
        self.seen = {e: {} for e in self.ENGS}
        self.last_w = {}
        self.readers = {}
        self.pending_pe = False

    def new_sem(self, name):
        self.nsem += 1
        h = self.stack.enter_context(self.nc.semaphore(f"{name}_{self.nsem}"))
        s = _Sem(h, name)
        self.allsems.append(s)
        return s

    def dsem(self, name):
        s = self.new_sem(name)
        return s

    DV = 256

    def _bank(self, k):
        if not (isinstance(k, tuple) and len(k) >= 2 and k[0] == "ps"):
            return None
        if isinstance(k[1], int):
            return k[1]
        if k[1] == "o":
            return k[2] * 2 + (k[3] * self.DV) // 512
        if k[1] == "st":
            return 4 + (k[2] * self.DV) // 512
        if k[1] in ("att", "n", "den"):
            return 6
        raise KeyError(k)

    def _expand(self, eng, reads, writes):
        r2, w2 = list(reads), list(writes)
        for k in reads:
            bk = self._bank(k)
            if bk is not None:
                assert eng != "pe"
                r2.append(("psbank", bk))
        for k in writes:
            bk = self._bank(k)
            if bk is not None:
                assert eng == "pe", (eng, k)
                w2.append(("psbank", bk))
        return r2, w2

    def _deps(self, eng, reads, writes):
        deps = {}

        def add(sv):
            s, v = sv
            if id(s) not in deps or deps[id(s)][1] < v:
                deps[id(s)] = (s, v)

        for k in reads:
            if k in self.last_w:
                add(self.last_w[k])
        for k in writes:
            if k in self.last_w:
                add(self.last_w[k])
            for sv in self.readers.get(k, ()):
                add(sv)
        out = []
        for s, v in deps.values():
            if eng == "pe" and s is self.esem["pe"]:
                continue
            if (not SAME_ENGINE_SYNC) and eng in self.esem and s is self.esem[eng]:
                continue
            if self.seen[eng].get(id(s), 0) >= v:
                continue
            self.seen[eng][id(s)] = v
            out.append((s.h, v))
        return out

    def _record(self, sv, reads, writes):
        for k in writes:
            self.last_w[k] = sv
            self.readers[k] = []
        for k in reads:
            self.readers.setdefault(k, []).append(sv)

    def op(self, eng, fn, reads=(), writes=(), sig=True):
        reads, writes = self._expand(eng, reads, writes)
        waits = self._deps(eng, reads, writes)
        s = self.esem[eng]
        if sig:
            s.v += 1
            val = s.v
            if eng == "pe":
                self.pending_pe = False
        else:
            assert eng == "pe"
            val = s.v + 1
            self.pending_pe = True
        self._record((s, val), reads, writes)
        sh = s.h

        def emit(E, waits=waits, fn=fn, sig=sig, sh=sh):
            for h, v in waits:
                E.wait_ge(h, v)
            inst = fn(E)
            if sig:
                inst.then_inc(sh, 1)

        self.q[eng].append(emit)

    def dma(self, queue, sem, out, in_, reads=(), writes=(), **kw):
        reads = list(reads)
        writes = list(writes)
        skey = ("dsem", reads[0] if reads else writes[0])
        if skey not in self.__dict__.setdefault("_dsems", {}):
            self._dsems[skey] = self.new_sem("d")
        sem = self._dsems[skey]
        waits = self._deps(queue, reads, writes)
        sem.v += 16
        self._record((sem, sem.v), reads, writes)
        sh = sem.h

        def emit(E, waits=waits, sh=sh, out=out, in_=in_, kw=kw):
            for h, v in waits:
                E.wait_ge(h, v)
            E.dma_start(out=out, in_=in_, **kw).then_inc(sh, 16)

        self.q[queue].append(emit)

    def custom(self, eng, fn, reads=(), writes=(), sem=None, inc=1):
        reads = list(reads)
        writes = list(writes)
        waits = self._deps(eng, reads, writes)
        sem.v += inc
        self._record((sem, sem.v), reads, writes)
        sh = sem.h

        def emit(E, waits=waits, fn=fn, sh=sh):
            for h, v in waits:
                E.wait_ge(h, v)
            fn(E, sh)

        self.q[eng].append(emit)

    def wait_keys(self, eng, keys):
        waits = self._deps(eng, list(keys), [])

        def emit(E, waits=waits):
            for h, v in waits:
                E.wait_ge(h, v)

        self.q[eng].append(emit)

    def barrier(self):
        assert not self.pending_pe
        for eng in ("pe", "act", "dve", "pool", "sp"):
            waits = []
            for s in self.allsems:
                if s.v > 0 and self.seen[eng].get(id(s), 0) < s.v and not (s is self.esem.get(eng)):
                    self.seen[eng][id(s)] = s.v
                    waits.append((s.h, s.v))

            def emit(E, waits=waits):
                for h, v in waits:
                    E.wait_ge(h, v)

            self.q[eng].append(emit)
        for eng in ("pe", "act", "dve", "pool"):
            if self.esem[eng].v > 24000:
                self.esem[eng] = self.new_sem("c_" + eng)

    def emit_all(self, block):
        assert not self.pending_pe
        q = self.q

        @block.tensor
        def _(E):
            for f in q["pe"]:
                f(E)

        @block.scalar
        def _(E):
            for f in q["act"]:
                f(E)

        @block.vector
        def _(E):
            for f in q["dve"]:
                f(E)

        @block.gpsimd
        def _(E):
            for f in q["pool"]:
                f(E)

        @block.sync
        def _(E):
            for f in q["sp"]:
                f(E)


def _consts():
    c = {}
    c["ident"] = np.eye(128, dtype=np.float32)
    s = np.arange(128)
    same = (s[:, None] // 64) == (s[None, :] // 64)
    sl = s % 64
    tric = np.zeros((128, 132), np.float32)
    tric[:, 0:128] = same * ((s[:, None] <= s[None, :]).astype(np.float32) - (sl[:, None] <= 31).astype(np.float32))
    for ch in range(2):
        tric[:, 128 + ch] = (s // 64 == ch)
        tric[:, 130 + ch] = (s // 64 == ch) & (sl <= 31)
    c["tric"] = tric
    c["suf"] = (same & (s[:, None] > s[None, :])).astype(np.float32)
    c["maskT"] = (sl[:, None] <= np.arange(64)[None, :]).astype(np.float32)
    c["ones"] = np.ones((128, 128), np.float32)
    sel = np.zeros((16, 16, 128), np.float32)
    for e in range(16):
        sel[e, e, :] = 1.0
    c["sel16"] = sel.reshape(16, 16 * 128)
    return c


def _fm_vec(v):
    v = np.asarray(v, np.float32)
    return np.ascontiguousarray(v.reshape(-1, 128).T)


def prepare_inputs(inp):
    f = lambda a: np.ascontiguousarray(np.asarray(a, dtype=np.float32))
    sh = dict(_consts())
    x = f(inp["x"])[0]
    pp = f(inp["p"])[:, 0]
    sh["rw"] = f(inp["router_w"])
    sh["rb"] = f(inp["router_b"]).reshape(1, 16)
    slotc = {"A": 0, "B": 0, "C": 0}
    for l in range(DEPTH):
        k = KINDS[l]
        s = slotc[k]
        slotc[k] += 1
        pre = f"L{l}_"
        vec = np.stack([_fm_vec(inp["ln1_g"][l]), _fm_vec(inp["ln1_b"][l]), _fm_vec(inp["ln2_g"][l]),
                        _fm_vec(inp["ln2_b"][l]),
                        _fm_vec({"A": inp["mlstm_norm_g"], "B": inp["gla_norm_g"], "C": inp["hgrn_norm_g"]}[k][s])],
                       axis=2)
        sh[pre + "vec"] = np.ascontiguousarray(vec)
        if k == "A":
            w = f(inp["mlstm_w_in"][s])
            sh[pre + "wqk"] = np.ascontiguousarray(w[:, 0:1024])
            sh[pre + "wv"] = np.ascontiguousarray(w[:, 1024:2048])
            sh[pre + "wg"] = np.ascontiguousarray(w[:, 2048:3072])
            sh[pre + "wgt"] = np.ascontiguousarray(w[:, 3072:3080])
            cw = f(inp["mlstm_conv"][s])
            sh[pre + "conv"] = np.ascontiguousarray(cw.reshape(4, 8, 128).transpose(2, 1, 0))
            sh[pre + "gb"] = f(inp["mlstm_gate_b"][s]).reshape(1, 8)
            sh[pre + "wout"] = f(inp["mlstm_w_out"][s])
        elif k == "B":
            w = f(inp["gla_w_in"][s])
            sh[pre + "wqk"] = np.ascontiguousarray(w[:, 0:1024])
            sh[pre + "wv"] = np.ascontiguousarray(w[:, 1024:2048])
            sh[pre + "wg"] = np.ascontiguousarray(w[:, 2048:3072])
            sh[pre + "wlr"] = np.ascontiguousarray(w[:, 3072:3088])
            sh[pre + "wa2"] = f(inp["gla_w_a2"][s])
            sh[pre + "ba"] = f(inp["gla_b_a"][s]).reshape(1, 512)
            sh[pre + "wout"] = f(inp["gla_w_out"][s])
        else:
            w = f(inp["hgrn_w_in"][s])
            sh[pre + "wqk"] = np.ascontiguousarray(w[:, 0:2048])
            sh[pre + "wv"] = np.ascontiguousarray(w[:, 2048:3072])
            sh[pre + "wg"] = np.ascontiguousarray(w[:, 3072:4096])
            lb = f(inp["hgrn_lb"])
            sh[pre + "lbT"] = np.ascontiguousarray(lb.reshape(DEPTH, 8, 128).transpose(2, 1, 0))
            sh[pre + "wout"] = f(inp["hgrn_w_out"][s])
        sh[pre + "eg"] = f(inp["exp_w_gate"][l])
        sh[pre + "eu"] = f(inp["exp_w_up"][l])
        sh[pre + "ed"] = f(inp["exp_w_down"][l])
        sh[pre + "pwp"] = f(inp["ple_w_proj"][l])
        sh[pre + "pwg"] = f(inp["ple_w_gate"][l])
    cores = []
    for c in range(NCORES):
        d = {}
        d["x"] = np.ascontiguousarray(x[c * T:(c + 1) * T])
        d["p"] = np.ascontiguousarray(pp[:, c * T:(c + 1) * T])
        hal = np.zeros((4, D), np.float32)
        if c > 0:
            hal[0:3] = x[c * T - 3:c * T]
        d["xhalo0"] = hal
        m = np.zeros((128, 16), np.float32)
        m[:, 0:8] = (np.arange(8) < c).astype(np.float32)[None, :]
        m[:, 8:16] = 1.0 - m[:, 0:8]
        d["cmask"] = m
        selm = np.zeros((32, 3), np.float32)
        if c > 0:
            for j in range(3):
                selm[(c - 1) * 4 + 1 + j, j] = 1.0
        d["selm"] = selm
        cores.append(d)
    return sh, cores


def build(n_layers=DEPTH, stop_after=None, shapes=None):
    nc = bass.Bass("TRN2", target_bir_lowering=False)
    dram = {}
    for name, shp in shapes.items():
        dram[name] = nc.dram_tensor(name, list(shp), F32, kind="ExternalInput").ap()
    out_d = nc.dram_tensor("out", [T, D], F32, kind="ExternalOutput").ap()
    CCW = 1024 + 16
    cc_in = nc.dram_tensor("cc_in", [128, CCW], F32)
    cc_out = nc.dram_tensor("cc_out", [NCORES * 128, CCW], F32)
    hc_in = nc.dram_tensor("hc_in", [4, D], F32)
    hc_out = nc.dram_tensor("hc_out", [NCORES * 4, D], F32)

    st = ExitStack()
    with st:
        sbt = lambda name, shape, dt: st.enter_context(nc.sbuf_tensor("sb_" + name, shape, dt))
        XT = sbt("XT", [128, 8, T], F32)
        PSt = st.enter_context(nc.psum_tensor("PS", [128, 8, 512], F32))
        ident = sbt("ident", [128, 128], F32)
        tric = sbt("tric", [128, 132], F32)
        sufm = sbt("sufm", [128, 128], F32)
        maskT = sbt("maskT", [128, 64], F32)
        ones = sbt("ones", [128, 128], F32)
        cmask = sbt("cmask", [128, 16], F32)
        vec = sbt("vec", [128, 8, 5], F32)
        rb_sb = sbt("rb_sb", [128, 16], F32)
        rw_sb = sbt("rw_sb", [128, 8, 16], F32)
        gates_all = sbt("gates_all", [128, NT, 16], F32)
        NF = 10600
        NH = 45200
        ARF = sbt("ARF", [128, NF], F32)
        ARH = sbt("ARH", [128, NH], BF16)

        p = Prog(nc, st)
        block = st.enter_context(nc.Block())

        class Arena:
            def __init__(self, t, n):
                self.t, self.n, self.off = t, n, 0

            def reset(self):
                self.off = 0

            def get(self, *shape):
                n = int(np.prod(shape))
                assert self.off + n <= self.n, (self.off, n, self.n)
                v = self.t[:, self.off:self.off + n]
                self.off += n
                if len(shape) == 2:
                    v = v.rearrange("p (a b) -> p a b", a=shape[0])
                elif len(shape) == 3:
                    v = v.rearrange("p (a b c) -> p a b c", a=shape[0], b=shape[1])
                return v

        af = Arena(ARF, NF)
        ah = Arena(ARH, NH)
        LN = {}

        def PS(b):
            return PSt[:, b, :]

        def mm(out, pairs, reads, writes):
            n = len(pairs)
            for i, (l_, r_) in enumerate(pairs):
                p.op("pe", lambda E, out=out, l_=l_, r_=r_, i=i, n=n: E.matmul(out, lhsT=l_, rhs=r_, start=(i == 0), stop=(i == n - 1)),
                     reads=reads, writes=writes, sig=(i == n - 1))

        def tr(out, in_, reads, writes):
            p.op("pe", lambda E: E.transpose(out, in_, ident[:]), reads=list(reads) + ["const"], writes=writes)

        def act(out, in_, func, reads, writes, bias=None, scale=None, accum=None):
            kw = {}
            if bias is not None:
                kw["bias"] = bias
            if scale is not None:
                kw["scale"] = scale
            if accum is not None:
                kw["accum_out"] = accum
            p.op("act", lambda E: E.activation(out=out, in_=in_, func=func, **kw), reads=reads, writes=writes)

        def tt(eng, out, a, b, op, reads, writes):
            p.op(eng, lambda E: E.tensor_tensor(out=out, in0=a, in1=b, op=op), reads=reads, writes=writes)

        def ts(eng, out, a, s1, op0, reads, writes, s2=None, op1=None):
            if op1 is None:
                p.op(eng, lambda E: E.tensor_scalar(out=out, in0=a, scalar1=s1, scalar2=None, op0=op0), reads=reads, writes=writes)
            else:
                p.op(eng, lambda E: E.tensor_scalar(out=out, in0=a, scalar1=s1, scalar2=s2, op0=op0, op1=op1), reads=reads, writes=writes)

        def stt(out, a, s, b, op0, op1, reads, writes):
            p.op("dve", lambda E: E.scalar_tensor_tensor(out=out, in0=a, scalar=s, in1=b, op0=op0, op1=op1), reads=reads, writes=writes)

        def cp(eng, out, in_, reads, writes):
            if eng == "act":
                act(out, in_, AF.Copy, reads, writes)
            else:
                p.op(eng, lambda E: E.tensor_copy(out=out, in_=in_), reads=reads, writes=writes)

        def red(out, in_, op, reads, writes):
            p.op("dve", lambda E: E.tensor_reduce(out=out, in_=in_, axis=AX.X, op=op), reads=reads, writes=writes)

        def recip(out, in_, reads, writes):
            p.op("dve", lambda E: E.reciprocal(out=out, in_=in_), reads=reads, writes=writes)

        def allgather(src, dst, reads, writes):
            p.custom("pool", lambda E, sh: E.collective_compute("AllGather", ALU.bypass, replica_groups=[list(range(NCORES))],
                                                                ins=[src.ap().opt()], outs=[dst.ap().opt()]).then_inc(sh),
                     reads=reads, writes=writes, sem=s_cc, inc=1)

        s_c = p.new_sem("ldc")
        for t_sb, nm in ((ident, "ident"), (tric, "tric"), (sufm, "suf"), (maskT, "maskT"), (ones, "ones"), (cmask, "cmask")):
            p.dma("sp", s_c, t_sb[:], dram[nm][:, :], writes=["const"])
        p.dma("sp", s_c, rw_sb[:], dram["rw"].rearrange("(kt p) e -> p kt e", p=128), writes=["const"])
        p.dma("sp", s_c, rb_sb[:], dram["rb"][0:1, :].broadcast_to([128, 16]), writes=["const"])

        wsems = [p.new_sem(f"w{i}") for i in range(8)]
        s_io = p.new_sem("io")
        s_io2 = p.new_sem("io2")
        s_cc = p.new_sem("cc")
        s_ccd = p.new_sem("ccd")
        s_out = p.new_sem("out")
        s_h = p.new_sem("halo")
        s_h2 = p.new_sem("halo2")
        s_sel = p.new_sem("selm")

        af.reset()
        xin = [af.get(1024), af.get(1024)]
        for ti in range(NT):
            xb = xin[ti % 2]
            p.dma("sp", s_io if ti % 2 == 0 else s_io2, xb, dram["x"][ti * 128:(ti + 1) * 128, :], writes=[("xin", ti % 2)])
            for kt in range(8):
                bank = (ti % 2) * 2 + kt // 4
                tr(PSt[:, bank, (kt % 4) * 128:(kt % 4 + 1) * 128], xb[:, kt * 128:(kt + 1) * 128],
                   reads=[("xin", ti % 2)], writes=[("ps", bank)])
            for hh in range(2):
                bank = (ti % 2) * 2 + hh
                cp("act" if hh == 0 else "dve", XT[:, hh * 4:(hh + 1) * 4, ti * 128:(ti + 1) * 128],
                   PSt[:, bank, :].rearrange("p (a b) -> p a b", a=4), reads=[("ps", bank)],
                   writes=[("XT", kt, ti // TPB) for kt in range(hh * 4, hh * 4 + 4)])
        p.barrier()
        n_layers_eff = 0 if stop_after == "load" else n_layers

        def layernorm_block(b, gi, bi, xT_out):
            c0 = b * TB
            sq = [LN["b"][0], LN["b"][1]]
            s1 = PS(6)
            s2 = PS(7)
            for kt in range(8):
                act(sq[kt % 2], XT[:, kt, c0:c0 + TB], AF.Square, reads=[("XT", kt, b)], writes=[("lnsq", kt % 2)])
                p.op("pe", lambda E, kt=kt: E.matmul(s1, lhsT=ones[:], rhs=XT[:, kt, c0:c0 + TB], start=(kt == 0), stop=(kt == 7)),
                     reads=[("XT", kt, b), "const"], writes=[("ps", 6)], sig=(kt == 7))
                p.op("pe", lambda E, kt=kt: E.matmul(s2, lhsT=ones[:], rhs=sq[kt % 2], start=(kt == 0), stop=(kt == 7)),
                     reads=[("lnsq", kt % 2), "const"], writes=[("ps", 7)], sig=True)
            mean, rstd, tmp = LN["b"][2], LN["b"][3], LN["b"][4]
            ts("dve", mean, s1, 1.0 / D, ALU.mult, reads=[("ps", 6)], writes=["ln_mean"])
            tt("dve", tmp, mean, mean, ALU.mult, reads=["ln_mean"], writes=["ln_tmp"])
            stt(rstd, s2, 1.0 / D, tmp, ALU.mult, ALU.subtract, reads=[("ps", 7), "ln_tmp"], writes=["ln_rstd"])
            ts("dve", rstd, rstd, LN_EPS, ALU.add, reads=["ln_rstd"], writes=["ln_rstd"])
            act(rstd, rstd, AF.Sqrt, reads=["ln_rstd"], writes=["ln_rstd"])
            recip(rstd, rstd, reads=["ln_rstd"], writes=["ln_rstd"])
            stt(mean, mean, -1.0, rstd, ALU.mult, ALU.mult, reads=["ln_mean", "ln_rstd"], writes=["ln_mean"])
            for kt in range(8):
                t1 = sq[kt % 2]
                tt("dve", t1, XT[:, kt, c0:c0 + TB], rstd, ALU.mult, reads=[("XT", kt, b), "ln_rstd"], writes=[("lnsq", kt % 2)])
                tt("pool", t1, t1, mean, ALU.add, reads=[("lnsq", kt % 2), "ln_mean"], writes=[("lnsq", kt % 2)])
                act(XT[:, kt, c0:c0 + TB], t1, AF.Identity, reads=[("lnsq", kt % 2), "vec"], writes=[("XT", kt, b)],
                    scale=vec[:, kt, gi:gi + 1], bias=vec[:, kt, bi:bi + 1])
                if xT_out is not None:
                    act(xT_out[:, kt, c0:c0 + TB], t1, AF.Identity, reads=[("lnsq", kt % 2), "vec"], writes=[("xT", kt, b)],
                        scale=vec[:, kt, gi:gi + 1], bias=vec[:, kt, bi:bi + 1])

        wslot_ctr = [0]

        def wload(slots, dram_ap, ncols, rows=1024):
            i = wslot_ctr[0] % len(slots)
            wslot_ctr[0] += 1
            buf, sem, key = slots[i]
            kts = rows // 128
            p.dma("pool", sem, buf[:, 0:kts, 0:ncols], dram_ap.rearrange("(kt p) f -> p kt f", p=128), writes=[key])
            return buf, key

        for l in range(n_layers_eff):
            kind = KINDS[l]
            cfg = CFG[kind]
            H, DV, lmul, gact = cfg["H"], cfg["DV"], cfg["lmul"], cfg["gact"]
            HD = H * 128
            p.DV = DV
            pre = f"L{l}_"
            W = lambda nm, pre=pre: dram[pre + nm]
            last = (l == n_layers - 1)
            af.reset()
            ah.reset()
            LAW = 512 if kind == "B" else 128
            LA = af.get(TPB, LAW)
            ESUF = af.get(TPB, 128)
            LI = af.get(TPB, 128) if kind == "A" else None
            GT = af.get(TPB, 8) if kind == "A" else None
            qe = af.get(TB)
            ke = af.get(TB)
            Eq = af.get(TB)
            Ek = af.get(TB)
            AUX = af.get(H, TPB, 4)
            Sst = af.get(H, DV)
            nst = af.get(8)
            Gtot = af.get(8)
            yb = af.get(1024)
            small = af.get(64)
            misc = af.get(TB)
            zb = af.get(TB + 4) if kind == "A" else None
            zhalo = af.get(8, 3) if kind == "A" else None
            convw = af.get(8, 4) if kind == "A" else None
            gb_sb = af.get(8) if kind == "A" else None
            lbv = af.get(8, DEPTH) if kind == "C" else None
            lb_l = af.get(8) if kind == "C" else None
            oml_l = af.get(8) if kind == "C" else None
            ba_f = af.get(512) if kind == "B" else None
            lnblk = af.get(5 * TB)
            LN["b"] = [lnblk[:, i * TB:(i + 1) * TB] for i in range(5)]
            rbuf = lnblk[:, 0:CCW]

            xTb = ah.get(8, TB)
            yTb = ah.get(8, TB)
            qtT = ah.get(H, TB)
            ktT = ah.get(H, TB)
            khat = ah.get(TPB, HD)
            vaug = ah.get(TPB, 1024)
            gate = ah.get(TPB, 1024)
            Sbf = ah.get(H, DV)
            nbf = ah.get(8)
            attT = ah.get(H, 64)
            ones_bf = ah.get(2)
            wsl = [(ah.get(8, 512), wsems[i], ("wblk", i)) for i in range(2)]
            alr = ah.get(TB) if kind == "B" else None
            wa2b = ah.get(512) if kind == "B" else None
            wgt_b = ah.get(8, 8) if kind == "A" else None
            xhT = ah.get(8, 4) if kind == "A" else None

            def lah(h):
                return LA[:, :, h * 128:(h + 1) * 128] if kind == "B" else LA[:, :, 0:128]

            p.dma("sp", s_c, vec[:], W("vec")[:, :, :], writes=["vec"])
            p.op("pool", lambda E, a=ones_bf: E.memset(a, 1.0), writes=["ones_bf"])
            if kind == "A":
                p.dma("sp", s_c, convw, W("conv")[:, :, :], writes=["lparam"])
                p.dma("sp", s_c, gb_sb, W("gb")[0:1, :].broadcast_to([128, 8]), writes=["lparam"])
                p.dma("pool", wsems[2], wgt_b, W("wgt").rearrange("(kt p) f -> p kt f", p=128), writes=["wgt"])
            if kind == "B":
                p.dma("pool", wsems[2], wa2b[0:16, :], W("wa2")[:, :], writes=["wa2"])
                p.dma("sp", s_c, ba_f[0:1, :], W("ba")[0:1, :], writes=["lparam"])
            if kind == "C":
                p.dma("sp", s_c, lbv, W("lbT")[:, :, :], writes=["lparam"])
                act(lbv, lbv, AF.Exp, reads=["lparam"], writes=["lparam"])
                red(lb_l, lbv, ALU.add, reads=["lparam"], writes=["lb_sum"])
                recip(lb_l, lb_l, reads=["lb_sum"], writes=["lb_sum"])
                if l >= 1:
                    red(oml_l, lbv[:, :, 1:l + 1], ALU.add, reads=["lparam"], writes=["lb_num"])
                else:
                    p.op("dve", lambda E, a=oml_l: E.memset(a, 0.0), writes=["lb_num"])
                tt("dve", lb_l, lb_l, oml_l, ALU.mult, reads=["lb_sum", "lb_num"], writes=["lb"])
                ts("dve", oml_l, lb_l, -1.0, ALU.mult, reads=["lb"], writes=["oml"], s2=1.0, op1=ALU.add)

            p.barrier()
            if kind == "A":
                hbuf, hbuf2 = LN["b"][0], LN["b"][1]
                if l == 0:
                    nrow = 4
                    p.dma("sp", s_h, hbuf[0:4, :], dram["xhalo0"][:, 0:512], writes=["hrow0"])
                    p.dma("sp", s_h2, hbuf2[0:4, :], dram["xhalo0"][:, 512:1024], writes=["hrow1"])
                    selt = small[0:4, 0:3]
                    cp("pool", selt, ident[0:4, 0:3], reads=["const"], writes=["selt"])
                else:
                    nrow = 32
                    p.dma("sp", s_h, hbuf[0:32, :], hc_out.ap()[:, 0:512], reads=["hc_out"], writes=["hrow0"])
                    p.dma("sp", s_h2, hbuf2[0:32, :], hc_out.ap()[:, 512:1024], reads=["hc_out"], writes=["hrow1"])
                    selt = small[0:32, 0:3]
                    p.dma("sp", s_sel, selt, dram["selm"][:, :], writes=["selt"])
                for kt in range(8):
                    src = (hbuf if kt < 4 else hbuf2)[0:nrow, (kt % 4) * 128:(kt % 4 + 1) * 128]
                    mm(PSt[:, 7, kt * 4:kt * 4 + 3], [(src, selt)], reads=["hrow0", "hrow1", "selt"], writes=[("ps", 7)])
                cp("act", xhT[:, :, 0:3], PSt[:, 7, 0:32].rearrange("p (a b) -> p a b", a=8)[:, :, 0:3], reads=[("ps", 7)], writes=["xhT"])
                p.barrier()

            def phase_a(b, mode):
                c0 = b * TB
                XTk = [("XT", kt, b) for kt in range(8)]
                cp("act", xTb[:, 0:4, :], XT[:, 0:4, c0:c0 + TB], reads=XTk[0:4], writes=["xTb0"])
                cp("pool", xTb[:, 4:8, :], XT[:, 4:8, c0:c0 + TB], reads=XTk[4:8], writes=["xTb1"])
                RX = ["xTb0", "xTb1"]

                def proj_fm(wbuf, wkey, col0, outps, pskey):
                    mm(outps, [(wbuf[:, kt, col0:col0 + 128], xTb[:, kt, :]) for kt in range(8)], reads=RX + [wkey], writes=[pskey])

                def proj_tm(wbuf, wkey, ti, ncol, outps, pskey):
                    mm(outps, [(xTb[:, kt, ti * 128:(ti + 1) * 128], wbuf[:, kt, 0:ncol]) for kt in range(8)], reads=RX + [wkey], writes=[pskey])

                if kind == "A":
                    for ti in range(TPB):
                        g8 = PSt[:, 6, ti * 8:ti * 8 + 8]
                        mm(g8, [(xTb[:, kt, ti * 128:(ti + 1) * 128], wgt_b[:, kt, :]) for kt in range(8)], reads=RX + ["wgt"], writes=[("ps", 6, ti)])
                        tt("dve", GT[:, ti, :], g8, gb_sb, ALU.add, reads=[("ps", 6, ti), "lparam"], writes=[("GT", ti)])
                        act(GT[:, ti, 4:8], GT[:, ti, 4:8], AF.Exp, reads=[("GT", ti)], writes=[("GT", ti)], scale=-1.0)
                        act(GT[:, ti, 4:8], GT[:, ti, 4:8], AF.Ln, reads=[("GT", ti)], writes=[("GT", ti)], bias=1.0)
                elif kind == "B":
                    wb_, wk_ = wload(wsl, W("wlr"), 16)
                    mm(PSt[0:16, 6, :], [(wb_[:, kt, 0:16], xTb[:, kt, :]) for kt in range(8)], reads=RX + [wk_], writes=[("ps", 6)])
                    cp("act", alr[0:16, :], PSt[0:16, 6, :], reads=[("ps", 6)], writes=["alr"])
                    for ti in range(TPB):
                        pj = PS(ti % 2)
                        p.op("pe", lambda E, pj=pj, l_=alr[0:16, ti * 128:(ti + 1) * 128], r_=wa2b[0:16, :]: E.matmul(pj, lhsT=l_, rhs=r_, start=True, stop=False),
                             reads=["alr", "wa2"], writes=[("ps", ti % 2)], sig=False)
                        p.op("pe", lambda E, pj=pj, l_=ones[0:1, :], r_=ba_f[0:1, :]: E.matmul(pj, lhsT=l_, rhs=r_, start=False, stop=True),
                             reads=["lparam", "const"], writes=[("ps", ti % 2)], sig=True)
                        act(misc, pj, AF.Exp, reads=[("ps", ti % 2)], writes=["misc"], scale=-1.0)
                        act(LA[:, ti, :], misc, AF.Ln, reads=["misc"], writes=["LA"], bias=1.0)

                def get_wblock(colblk):
                    return wload(wsl, W("wqk")[:, colblk * 512:(colblk + 1) * 512], 512)

                wcur = [None, 0, None]

                def conv_silu(h, is_k, src_ps, pskey, dst, scale_after):
                    ct = (4 if is_k else 0) + h
                    cp("act", zb[:, 3:3 + TB], src_ps, reads=[pskey], writes=["zb"])
                    if b == 0:
                        mm(PSt[:, 3, 64:67], [(wcur[0][:, kt, wcur[1]:wcur[1] + 128], xhT[:, kt, 0:3]) for kt in range(8)],
                           reads=["xhT", wcur[2]], writes=[("ps", 3, "h")])
                        cp("dve", zb[:, 0:3], PSt[:, 3, 64:67], reads=[("ps", 3, "h")], writes=["zb"])
                    else:
                        cp("dve", zb[:, 0:3], zhalo[:, ct, :], reads=[("zhalo", ct)], writes=["zb"])
                    cp("pool", zhalo[:, ct, :], zb[:, TB:TB + 3], reads=["zb"], writes=[("zhalo", ct)])
                    kq = ("qk", is_k)
                    ts("dve", dst, zb[:, 0:TB], convw[:, ct, 0:1], ALU.mult, reads=["zb", "lparam"], writes=[kq])
                    for w in range(1, 4):
                        stt(dst, zb[:, w:w + TB], convw[:, ct, w:w + 1], dst, ALU.mult, ALU.add, reads=["zb", "lparam", kq], writes=[kq])
                    if scale_after != 1.0:
                        act(misc, dst, AF.Silu, reads=[kq], writes=["misc"])
                        ts("pool", dst, misc, scale_after, ALU.mult, reads=["misc"], writes=[kq])
                    else:
                        act(dst, dst, AF.Silu, reads=[kq], writes=[kq])

                for h in range(H):
                    if kind in ("A", "B"):
                        if h == 0:
                            wkb, wkk = get_wblock(1)
                        wcur[0], wcur[1], wcur[2] = wkb, h * 128, wkk
                        proj_fm(wkb, wkk, h * 128, PS(0), ("ps", 0))
                        if kind == "A":
                            conv_silu(h, 1, PS(0), ("ps", 0), ke, 128 ** -0.5)
                            cp("dve", LA[:, :, 0:128], GT[:, :, 4 + h:5 + h].broadcast_to([128, TPB, 128]), reads=[("GT", ti) for ti in range(TPB)], writes=["LA"])
                            cp("dve", LI[:, :, :], GT[:, :, h:h + 1].broadcast_to([128, TPB, 128]), reads=[("GT", ti) for ti in range(TPB)], writes=["LI"])
                        else:
                            cp("act", ke, PS(0), reads=[("ps", 0)], writes=[("qk", 1)])
                    else:
                        if h % 4 == 0:
                            wkb, wkk = get_wblock(2 + h // 4)
                        proj_fm(wkb, wkk, (h % 4) * 128, PS(0), ("ps", 0))
                        act(misc, PS(0), AF.Sigmoid, reads=[("ps", 0)], writes=["misc"])
                        ts("dve", misc, misc, oml_l[:, h:h + 1], ALU.mult, reads=["misc", "oml", "lb"], writes=["misc"], s2=lb_l[:, h:h + 1], op1=ALU.add)
                        ts("pool", ke, misc, -1.0, ALU.mult, reads=["misc"], writes=[("qk", 1)], s2=1.0, op1=ALU.add)
                        act(misc, misc, AF.Ln, reads=["misc"], writes=["misc"])
                        for ti in range(TPB):
                            tr(PSt[:, 6, ti * 128:(ti + 1) * 128], misc[:, ti * 128:(ti + 1) * 128], reads=["misc"], writes=[("ps", 6)])
                        cp("dve", LA[:, :, 0:128], PSt[:, 6, :].rearrange("p (a b) -> p a b", a=TPB), reads=[("ps", 6)], writes=["LA"])
                    la_h = lah(h)
                    for ti in range(TPB):
                        mm(PSt[:, 5, ti * 128:(ti + 1) * 128], [(sufm[:], la_h[:, ti, :])], reads=["LA", "const"], writes=[("ps", 5)])
                    ps5 = PSt[:, 5, :].rearrange("p (a b) -> p a b", a=TPB)
                    if kind == "A":
                        stt(ESUF, ps5, lmul, LI, ALU.mult, ALU.add, reads=[("ps", 5), "LI"], writes=["ESUF"])
                        act(ESUF, ESUF, AF.Exp, reads=["ESUF"], writes=["ESUF"])
                    else:
                        act(ESUF, ps5, AF.Exp, reads=[("ps", 5)], writes=["ESUF"], scale=lmul)
                    for ti in range(TPB):
                        tr(PSt[:, 4, ti * 128:(ti + 1) * 128], ke[:, ti * 128:(ti + 1) * 128], reads=[("qk", 1)], writes=[("ps", 4)])
                    tt("dve", khat[:, :, h * 128:(h + 1) * 128], PSt[:, 4, :].rearrange("p (a b) -> p a b", a=TPB), ESUF,
                       ALU.mult, reads=[("ps", 4), "ESUF"], writes=[("khat", h)])
                    for ti in range(TPB):
                        lhs = la_h[:, ti, :]
                        mm(PSt[:, 3, ti * 4:ti * 4 + 4], [(lhs, tric[:, 128:132])], reads=["LA", "const"], writes=[("ps", 3)])
                        if mode == "main":
                            mm(PSt[:, 2, ti * 128:(ti + 1) * 128], [(lhs, tric[:, 0:128])], reads=["LA", "const"], writes=[("ps", 2)])
                            if kind == "A":
                                mm(PSt[:, 7, ti * 128:(ti + 1) * 128], [(lhs, tric[:, 0:128]), (LI[:, ti, :], ident[:])],
                                   reads=["LA", "LI", "const"], writes=[("ps", 7)])
                    act(AUX[:, h, :, :], PSt[:, 3, 0:16].rearrange("p (a b) -> p a b", a=TPB), AF.Exp, reads=[("ps", 3)], writes=[("AUX", h)], scale=lmul)
                    if mode == "main":
                        act(Eq, PS(2), AF.Exp, reads=[("ps", 2)], writes=["Eq"], scale=lmul)
                        kk = ("ps", 7) if kind == "A" else ("ps", 2)
                        act(Ek, PS(7) if kind == "A" else PS(2), AF.Exp, reads=[kk], writes=["Ek"], scale=-lmul)
                        tt("pool", ktT[:, h, :], ke, Ek, ALU.mult, reads=[("qk", 1), "Ek"], writes=[("ktT", h)])
                        if kind in ("A", "B"):
                            if h == 0:
                                wqb, wqk_ = get_wblock(0)
                            wcur[0], wcur[1], wcur[2] = wqb, h * 128, wqk_
                            proj_fm(wqb, wqk_, h * 128, PS(1), ("ps", 1))
                            if kind == "A":
                                conv_silu(h, 0, PS(1), ("ps", 1), qe, 1.0)
                            else:
                                act(qe, PS(1), AF.Copy, reads=[("ps", 1)], writes=[("qk", 0)], scale=128 ** -0.5)
                        else:
                            if h % 4 == 0:
                                wqb, wqk_ = get_wblock(h // 4)
                            proj_fm(wqb, wqk_, (h % 4) * 128, PS(1), ("ps", 1))
                            act(qe, PS(1), AF.Silu, reads=[("ps", 1)], writes=[("qk", 0)])
                        tt("dve", qtT[:, h, :], qe, Eq, ALU.mult, reads=[("qk", 0), "Eq"], writes=[("qtT", h)])

                for half in range(2):
                    wvb, wvk = wload(wsl, W("wv")[:, half * 512:(half + 1) * 512], 512)
                    for ti in range(TPB):
                        proj_tm(wvb, wvk, ti, 512, PS(ti % 2), ("ps", ti % 2))
                        cp("act" if ti % 2 == 0 else "dve", vaug[:, ti, half * 512:(half + 1) * 512], PS(ti % 2), reads=[("ps", ti % 2)], writes=[("vaug", ti)])
                if mode == "main":
                    for half in range(2):
                        wgb, wgk = wload(wsl, W("wg")[:, half * 512:(half + 1) * 512], 512)
                        for ti in range(TPB):
                            proj_tm(wgb, wgk, ti, 512, PS(ti % 2), ("ps", ti % 2))
                            act(gate[:, ti, half * 512:(half + 1) * 512], PS(ti % 2), gact, reads=[("ps", ti % 2)], writes=[("gate", ti)])

            def state_step(ci, h):
                ti, half = ci // 2, ci % 2
                p0 = half * 64
                stp = PSt[:, 4 + (h * DV) // 512, (h * DV) % 512:(h * DV) % 512 + DV]
                kh = khat[p0:p0 + 64, ti, h * 128:(h + 1) * 128]
                mm(stp, [(kh, vaug[p0:p0 + 64, ti, h * DV:(h + 1) * DV])], reads=[("khat", h), ("vaug", ti)], writes=[("ps", "st", h)])
                stt(Sst[:, h, :], Sst[:, h, :], AUX[:, h, ti, half:half + 1], stp, ALU.mult, ALU.add,
                    reads=[("S", h), ("AUX", h), ("ps", "st", h)], writes=[("S", h)])
                if kind == "A":
                    npz = PSt[:, 6, 256 + h:256 + h + 1]
                    mm(npz, [(kh, ones_bf[p0:p0 + 64, 0:1])], reads=[("khat", h), "ones_bf"], writes=[("ps", "n", h)])
                    stt(nst[:, h:h + 1], nst[:, h:h + 1], AUX[:, h, ti, half:half + 1], npz, ALU.mult, ALU.add,
                        reads=[("n", h), ("AUX", h), ("ps", "n", h)], writes=[("n", h)])

            def prepass_block(b):
                for ci in range(2 * TPB):
                    ti, half = ci // 2, ci % 2
                    for h in range(H):
                        state_step(ci, h)
                        ts("pool", Gtot[:, h:h + 1], Gtot[:, h:h + 1], AUX[:, h, ti, half:half + 1], ALU.mult, reads=[("G", h), ("AUX", h)], writes=[("G", h)])

            def out_stage(ti):
                ob = (ti % 2) * 2
                ss = small[:, 16:16 + H]
                rr = small[:, 32:32 + H]
                dn = small[:, 48:48 + H]

                def opsum(h):
                    return PSt[:, ob + (h * DV) // 512, (h * DV) % 512:(h * DV) % 512 + DV]

                for h in range(H):
                    act(yb[:, h * DV:(h + 1) * DV], opsum(h), AF.Square, reads=[("ps", "o", ti % 2, h, 0), ("ps", "o", ti % 2, h, 1)], writes=["yb", ("ss", h)],
                        accum=ss[:, h:h + 1])
                SSK = [("ss", h) for h in range(H)]
                if kind == "A":
                    cp("dve", dn, PSt[:, 6, 300:300 + H], reads=[("ps", "den", h, hf) for h in range(H) for hf in range(2)], writes=["dn"])
                    stt(dn, dn, -1.0, dn, ALU.mult, ALU.max, reads=["dn"], writes=["dn"])
                    ts("dve", dn, dn, 1.0, ALU.max, reads=["dn"], writes=["dn"])
                    recip(dn, dn, reads=["dn"], writes=["dn"])
                    tt("dve", rr, dn, dn, ALU.mult, reads=["dn"], writes=["rr"])
                    tt("dve", ss, ss, rr, ALU.mult, reads=SSK + ["rr"], writes=SSK)
                ts("dve", ss, ss, 1.0 / DV, ALU.mult, reads=SSK, writes=SSK, s2=LN_EPS, op1=ALU.add)
                act(ss, ss, AF.Sqrt, reads=SSK, writes=SSK)
                recip(rr, ss, reads=SSK, writes=["rr"])
                if kind == "A":
                    tt("dve", rr, rr, dn, ALU.mult, reads=["rr", "dn"], writes=["rr"])
                for h in range(H):
                    stt(yb[:, h * DV:(h + 1) * DV], opsum(h), rr[:, h:h + 1], gate[:, ti, h * DV:(h + 1) * DV], ALU.mult, ALU.mult,
                        reads=["rr", ("gate", ti), ("ps", "o", ti % 2, h, 0), ("ps", "o", ti % 2, h, 1)], writes=["yb"])
                for kt in range(8):
                    tps = PSt[:, 7, (kt % 4) * 128:(kt % 4 + 1) * 128]
                    tr(tps, yb[:, kt * 128:(kt + 1) * 128], reads=["yb"], writes=[("ps", 7, kt % 4)])
                    act(yTb[:, kt, ti * 128:(ti + 1) * 128], tps, AF.Copy, reads=[("ps", 7, kt % 4), "vec"], writes=[("yTb", ti)],
                        scale=vec[:, kt, 4:5])

            def phase_b(b):
                c0 = b * TB
                for ci in range(2 * TPB):
                    ti, half = ci // 2, ci % 2
                    p0 = half * 64
                    cc = slice(ci * 64, ci * 64 + 64)
                    ob = (ti % 2) * 2
                    for h in range(H):
                        act(Sbf[:, h, :], Sst[:, h, :], AF.Copy, reads=[("S", h), ("AUX", h)], writes=[("Sbf", h)], scale=AUX[:, h, ti, 2 + half:3 + half])
                        if kind == "A":
                            act(nbf[:, h:h + 1], nst[:, h:h + 1], AF.Copy, reads=[("n", h), ("AUX", h)], writes=[("nbf", h)], scale=AUX[:, h, ti, 2 + half:3 + half])
                        aps = PSt[p0:p0 + 64, 6, h * 64:(h + 1) * 64] if kind != "A" else PSt[p0:p0 + 64, 6, h * 64:(h + 1) * 64]
                        akey = ("ps", "att", h, half)
                        mm(aps, [(ktT[:, h, cc], qtT[:, h, cc])], reads=[("ktT", h), ("qtT", h)], writes=[akey])
                        tt("dve", attT[p0:p0 + 64, h, :], aps, maskT[p0:p0 + 64, :], ALU.mult, reads=[akey, "const"], writes=[("attT", h, half)])
                        ops_ = PSt[p0:p0 + 64, ob + (h * DV) // 512, (h * DV) % 512:(h * DV) % 512 + DV]
                        okey = ("ps", "o", ti % 2, h, half)
                        mm(ops_, [(attT[p0:p0 + 64, h, :], vaug[p0:p0 + 64, ti, h * DV:(h + 1) * DV]), (qtT[:, h, cc], Sbf[:, h, :])],
                           reads=[("attT", h, half), ("vaug", ti), ("qtT", h), ("Sbf", h)], writes=[okey])
                        if kind == "A":
                            dps = PSt[p0:p0 + 64, 6, 300 + h:301 + h]
                            mm(dps, [(attT[p0:p0 + 64, h, :], ones_bf[p0:p0 + 64, 0:1]), (qtT[:, h, cc], nbf[:, h:h + 1])],
                               reads=[("attT", h, half), "ones_bf", ("qtT", h), ("nbf", h)], writes=[("ps", "den", h, half)])
                        state_step(ci, h)
                    if half == 1:
                        out_stage(ti)
                for half in range(2):
                    wob, wok = wload(wsl, W("wout")[:, half * 512:(half + 1) * 512], 512)
                    for dd in range(4):
                        dt_ = half * 4 + dd
                        mp = PS(7)
                        k7 = [("ps", 7, k) for k in range(4)]
                        mm(mp, [(wob[:, kt, dd * 128:(dd + 1) * 128], yTb[:, kt, :]) for kt in range(8)], reads=[wok] + [("yTb", t) for t in range(TPB)], writes=k7)
                        stt(XT[:, dt_, c0:c0 + TB], XT[:, dt_, c0:c0 + TB], ALPHA, mp, ALU.mult, ALU.add, reads=[("XT", dt_, b)] + k7, writes=[("XT", dt_, b)])

            SK = [("S", h) for h in range(H)]
            NK = [("n", h) for h in range(8)]
            GK = [("G", h) for h in range(8)]
            p.op("dve", lambda E, a=Sst: E.memset(a, 0.0), writes=SK)
            p.op("dve", lambda E, a=nst: E.memset(a, 0.0), writes=NK)
            p.op("dve", lambda E, a=Gtot: E.memset(a, 1.0), writes=GK)
            for b in range(NB):
                phase_a(b, "pre")
                p.barrier()
                prepass_block(b)
                p.barrier()
            cp("dve", rbuf[:, 0:1024], Sst.rearrange("p a b -> p (a b)"), reads=SK, writes=["rbuf"])
            cp("dve", rbuf[:, 1024:1032], nst, reads=NK, writes=["rbuf"])
            cp("dve", rbuf[:, 1032:1040], Gtot, reads=GK, writes=["rbuf"])
            p.dma("sp", s_ccd, cc_in.ap()[:, :], rbuf, reads=["rbuf"], writes=["cc_in"])
            allgather(cc_in, cc_out, reads=["cc_in"], writes=["cc_out"])
            p.op("dve", lambda E, a=Sst: E.memset(a, 0.0), reads=["rbuf"], writes=SK)
            p.op("dve", lambda E, a=nst: E.memset(a, 0.0), reads=["rbuf"], writes=NK)
            geff = small[:, 0:8]
            for c2 in range(NCORES):
                p.dma("sp", s_ccd, rbuf, cc_out.ap()[c2 * 128:(c2 + 1) * 128, :], reads=["cc_out"], writes=["rbuf"])
                m_ap = cmask[:, c2:c2 + 1]
                om_ap = cmask[:, 8 + c2:9 + c2]
                ts("dve", geff, rbuf[:, 1032:1040], m_ap, ALU.mult, reads=["rbuf", "const"], writes=["geff"], s2=om_ap, op1=ALU.add)
                for h in range(H):
                    ts("pool", misc[:, 0:DV], rbuf[:, h * DV:(h + 1) * DV], m_ap, ALU.mult, reads=["rbuf", "const"], writes=["misc"])
                    stt(Sst[:, h, :], Sst[:, h, :], geff[:, h:h + 1], misc[:, 0:DV], ALU.mult, ALU.add, reads=[("S", h), "geff", "misc"], writes=[("S", h)])
                if kind == "A":
                    ts("pool", misc[:, 0:8], rbuf[:, 1024:1032], m_ap, ALU.mult, reads=["rbuf", "const"], writes=["misc"])
                    tt("dve", nst, nst, geff, ALU.mult, reads=NK + ["geff"], writes=NK)
                    tt("dve", nst, nst, misc[:, 0:8], ALU.add, reads=NK + ["misc"], writes=NK)
            p.barrier()
            if stop_after == "prepass" and last:
                break
            for b in range(NB):
                phase_a(b, "main")
                p.barrier()
                phase_b(b)
                p.barrier()
                layernorm_block(b, 0, 1, None)
                p.barrier()
            if stop_after == "ln1" and last:
                break

            af.reset()
            ah.reset()
            sel16 = af.get(16, 128)
            gT = af.get(T)
            Gsb = [af.get(TB), af.get(TB)]
            sg = [af.get(TB), af.get(TB)]
            rt = af.get(256)
            xT = ah.get(8, T)
            ew = [dict(g=ah.get(8, 512), u=ah.get(8, 512), d=ah.get(4, 1024), sem=wsems[3 + i]) for i in range(2)]
            hT = [ah.get(4, TB), ah.get(4, TB)]
            p.dma("sp", s_c, sel16[0:16, :, :], dram["sel16"].rearrange("k (e m) -> k e m", e=16), writes=["sel16"])

            def load_expert(e):
                s = ew[e % 2]
                key = ("ew", e % 2)
                p.dma("pool", s["sem"], s["g"], W("eg")[e].rearrange("(kt p) f -> p kt f", p=128), writes=[key])
                p.dma("pool", s["sem"], s["u"], W("eu")[e].rearrange("(kt p) f -> p kt f", p=128), writes=[key])
                p.dma("pool", s["sem"], s["d"], W("ed")[e].rearrange("(ft p) d -> p ft d", p=128), writes=[key])

            load_expert(0)
            for b in range(NB):
                c0 = b * TB
                for kt in range(8):
                    cp("act" if kt % 2 == 0 else "pool", xT[:, kt, c0:c0 + TB], XT[:, kt, c0:c0 + TB], reads=[("XT", kt, b)], writes=[("xT", kt, b)])
                for t4 in range(TPB):
                    ti = b * TPB + t4
                    lg = PSt[:, 5, t4 * 16:t4 * 16 + 16]
                    mm(lg, [(XT[:, kt, ti * 128:(ti + 1) * 128], rw_sb[:, kt, :]) for kt in range(8)], reads=[("XT", kt, b) for kt in range(8)] + ["const"], writes=[("ps", 5, t4)])
                    R = lambda a, n: rt[:, a:a + n]
                    mx, se, m1, m2 = R(0, 1), R(1, 1), R(2, 1), R(3, 1)
                    ex, sc, sel, selm, t16, eq, w16 = R(16, 16), R(32, 16), R(48, 16), R(80, 16), R(96, 16), R(112, 16), R(128, 16)
                    gs, ing, eq4, t4v = R(64, 4), R(68, 4), R(144, 4), R(148, 4)
                    k = "rt"
                    red(mx, lg, ALU.max, reads=[("ps", 5, t4)], writes=[k])
                    ts("dve", mx, mx, -1.0, ALU.mult, reads=[k], writes=[k])
                    act(ex, lg, AF.Exp, reads=[("ps", 5, t4), k], writes=[k], bias=mx, accum=se)
                    recip(se, se, reads=[k], writes=[k])
                    ts("dve", sc, ex, se, ALU.mult, reads=[k], writes=[k])
                    tt("dve", sel, sc, rb_sb[:], ALU.add, reads=[k, "const"], writes=[k])
                    for g in range(4):
                        s4 = sel[:, 4 * g:4 * g + 4]
                        red(m1, s4, ALU.max, reads=[k], writes=[k])
                        ts("dve", eq4, s4, m1, ALU.is_equal, reads=[k], writes=[k])
                        stt(t4v, eq4, -BIG, s4, ALU.mult, ALU.add, reads=[k], writes=[k])
                        red(m2, t4v, ALU.max, reads=[k], writes=[k])
                        tt("dve", gs[:, g:g + 1], m1, m2, ALU.add, reads=[k], writes=[k])
                    red(m1, gs, ALU.max, reads=[k], writes=[k])
                    ts("dve", ing, gs, m1, ALU.is_equal, reads=[k], writes=[k])
                    ts("dve", ing, ing, -1.0, ALU.add, reads=[k], writes=[k], s2=BIG, op1=ALU.mult)
                    for g in range(4):
                        ts("dve", selm[:, 4 * g:4 * g + 4], sel[:, 4 * g:4 * g + 4], ing[:, g:g + 1], ALU.add, reads=[k], writes=[k])
                    red(m1, selm, ALU.max, reads=[k], writes=[k])
                    ts("dve", eq, selm, m1, ALU.is_equal, reads=[k], writes=[k])
                    stt(t16, eq, -BIG, selm, ALU.mult, ALU.add, reads=[k], writes=[k])
                    red(m2, t16, ALU.max, reads=[k], writes=[k])
                    ts("dve", t16, t16, m2, ALU.is_equal, reads=[k], writes=[k])
                    tt("dve", eq, eq, t16, ALU.add, reads=[k], writes=[k])
                    tt("dve", w16, sc, eq, ALU.mult, reads=[k], writes=[k])
                    red(m1, w16, ALU.add, reads=[k], writes=[k])
                    recip(m1, m1, reads=[k], writes=[k])
                    ts("dve", gates_all[:, ti, :], w16, m1, ALU.mult, reads=[k], writes=[("gates", ti)])
                    tr(PSt[0:16, 4, t4 * 128:(t4 + 1) * 128], gates_all[:, ti, :], reads=[("gates", ti)], writes=[("ps", 4, t4)])
                cp("act", gT[0:16, c0:c0 + TB], PSt[0:16, 4, :], reads=[("ps", 4, t4) for t4 in range(TPB)], writes=[("gT", b)])
                for kt in range(8):
                    ts("pool", XT[:, kt, c0:c0 + TB], XT[:, kt, c0:c0 + TB], ALPHA, ALU.mult, reads=[("XT", kt, b)], writes=[("XT", kt, b)])
            p.barrier()

            for e in range(NEXP):
                if e + 1 < NEXP:
                    load_expert(e + 1)
                s = ew[e % 2]
                wkey = ("ew", e % 2)
                for b in range(NB):
                    c0 = b * TB
                    it = e * NB + b
                    G = Gsb[it % 2]
                    mm(PS(6), [(sel16[0:16, e, :], gT[0:16, c0:c0 + TB])], reads=["sel16", ("gT", b)], writes=[("ps", 6)])
                    cp("act", G, PS(6), reads=[("ps", 6)], writes=[("Gs", it % 2)])
                    hTb = hT[it % 2]
                    XK = [("xT", kt, b) for kt in range(8)]
                    for ft in range(4):
                        gp = PS(ft % 2)
                        up = PS(2 + ft % 2)
                        mm(gp, [(s["g"][:, kt, ft * 128:(ft + 1) * 128], xT[:, kt, c0:c0 + TB]) for kt in range(8)], reads=[wkey] + XK, writes=[("ps", ft % 2)])
                        mm(up, [(s["u"][:, kt, ft * 128:(ft + 1) * 128], xT[:, kt, c0:c0 + TB]) for kt in range(8)], reads=[wkey] + XK, writes=[("ps", 2 + ft % 2)])
                        sgb = sg[ft % 2]
                        act(sgb, gp, AF.Silu, reads=[("ps", ft % 2)], writes=[("sg", ft % 2)])
                        tt("pool", sgb, sgb, G, ALU.mult, reads=[("sg", ft % 2), ("Gs", it % 2)], writes=[("sg", ft % 2)])
                        tt("dve", hTb[:, ft, :], sgb, up, ALU.mult, reads=[("sg", ft % 2), ("ps", 2 + ft % 2)], writes=[("hT", it % 2, ft)])
                    for dt_ in range(8):
                        yp = PS(4 + dt_ % 2)
                        yk = ("ps", 4 + dt_ % 2)
                        mm(yp, [(s["d"][:, ft, dt_ * 128:(dt_ + 1) * 128], hTb[:, ft, :]) for ft in range(4)],
                           reads=[wkey] + [("hT", it % 2, ft) for ft in range(4)], writes=[yk])
                        tt("dve", XT[:, dt_, c0:c0 + TB], XT[:, dt_, c0:c0 + TB], yp, ALU.add, reads=[("XT", dt_, b), yk], writes=[("XT", dt_, b)])
            p.barrier()
            if stop_after == "moe" and last:
                break
            af.reset()
            ah.reset()
            lnblk = af.get(5 * TB)
            LN["b"] = [lnblk[:, i * TB:(i + 1) * TB] for i in range(5)]
            pin = [af.get(256), af.get(256)]
            sgm = [af.get(TB), af.get(TB)]
            xT = ah.get(8, T)
            wpg = ah.get(8, 1024)
            wpp = ah.get(2, 1024)
            pT = ah.get(2, TB)
            p.dma("pool", wsems[5], wpg, W("pwg").rearrange("(kt p) f -> p kt f", p=128), writes=["wpg"])
            p.dma("pool", wsems[6], wpp, W("pwp").rearrange("(kt p) f -> p kt f", p=128), writes=["wpp"])
            for b in range(NB):
                c0 = b * TB
                layernorm_block(b, 2, 3, xT)
                for t4 in range(TPB):
                    ti = b * TPB + t4
                    pb = pin[ti % 2]
                    p.dma("sp", s_io if ti % 2 == 0 else s_io2, pb, dram["p"][l, ti * 128:(ti + 1) * 128, :], writes=[("pin", ti % 2)])
                    for k2 in range(2):
                        tr(PSt[:, 4 + k2, t4 * 128:(t4 + 1) * 128], pb[:, k2 * 128:(k2 + 1) * 128], reads=[("pin", ti % 2)], writes=[("ps", 4 + k2, t4)])
                for k2 in range(2):
                    cp("act", pT[:, k2, :], PSt[:, 4 + k2, :], reads=[("ps", 4 + k2, t4) for t4 in range(TPB)], writes=[("pT", k2)])
                for dt_ in range(8):
                    gp = PS(dt_ % 2)
                    pp_ = PS(2 + dt_ % 2)
                    mm(gp, [(wpg[:, kt, dt_ * 128:(dt_ + 1) * 128], xT[:, kt, c0:c0 + TB]) for kt in range(8)],
                       reads=["wpg"] + [("xT", kt, b) for kt in range(8)], writes=[("ps", dt_ % 2)])
                    mm(pp_, [(wpp[:, k2, dt_ * 128:(dt_ + 1) * 128], pT[:, k2, :]) for k2 in range(2)],
                       reads=["wpp", ("pT", 0), ("pT", 1)], writes=[("ps", 2 + dt_ % 2)])
                    sb_ = sgm[dt_ % 2]
                    act(sb_, gp, AF.Sigmoid, reads=[("ps", dt_ % 2)], writes=[("sgm", dt_ % 2)])
                    tt("dve", sb_, sb_, pp_, ALU.mult, reads=[("sgm", dt_ % 2), ("ps", 2 + dt_ % 2)], writes=[("sgm", dt_ % 2)])
                    tt("pool", XT[:, dt_, c0:c0 + TB], XT[:, dt_, c0:c0 + TB], sb_, ALU.add, reads=[("XT", dt_, b), ("sgm", dt_ % 2)], writes=[("XT", dt_, b)])
            if l + 1 < n_layers and KINDS[l + 1] == "A":
                hb, hb2 = LN["b"][0], LN["b"][1]
                for kt in range(8):
                    tr(PSt[0:4, 6 + kt // 4, (kt % 4) * 128:(kt % 4 + 1) * 128], XT[:, kt, T - 4:T], reads=[("XT", kt, NB - 1)], writes=[("ps", 6 + kt // 4)])
                cp("act", hb[0:4, :], PSt[0:4, 6, :], reads=[("ps", 6)], writes=["hsend0"])
                cp("act", hb2[0:4, :], PSt[0:4, 7, :], reads=[("ps", 7)], writes=["hsend1"])
                p.dma("sp", s_h, hc_in.ap()[:, 0:512], hb[0:4, :], reads=["hsend0"], writes=["hc_in0"])
                p.dma("sp", s_h2, hc_in.ap()[:, 512:1024], hb2[0:4, :], reads=["hsend1"], writes=["hc_in1"])
                allgather(hc_in, hc_out, reads=["hc_in0", "hc_in1"], writes=["hc_out"])
            p.barrier()

        af.reset()
        xo = [af.get(1024), af.get(1024)]
        for ti in range(NT):
            b = ti // TPB
            for kt in range(8):
                bank = (ti % 2) * 2 + kt // 4
                tr(PSt[:, bank, (kt % 4) * 128:(kt % 4 + 1) * 128], XT[:, kt, ti * 128:(ti + 1) * 128], reads=[("XT", kt, b)], writes=[("ps", bank)])
            ob_ = xo[ti % 2]
            cp("act", ob_[:, 0:512], PSt[:, (ti % 2) * 2, :], reads=[("ps", (ti % 2) * 2)], writes=[("xo", ti % 2)])
            cp("dve", ob_[:, 512:1024], PSt[:, (ti % 2) * 2 + 1, :], reads=[("ps", (ti % 2) * 2 + 1)], writes=[("xo", ti % 2)])
            p.dma("sp", s_out, out_d[ti * 128:(ti + 1) * 128, :], ob_, reads=[("xo", ti % 2)], writes=[("out", ti)])
        p.wait_keys("sp", [("out", ti) for ti in range(NT)])
        p.emit_all(block)
    return nc


af_ln = None
_CACHE = {}


def run(inputs, n_layers=DEPTH, stop_after=None, trace=False):
    sh, cores = prepare_inputs(inputs)
    in_maps = []
    for c in range(NCORES):
        d = dict(sh)
        d.update(cores[c])
        in_maps.append(d)
    shapes = {k: v.shape for k, v in in_maps[0].items()}
    key = (n_layers, stop_after)
    if key not in _CACHE:
        _CACHE[key] = build(n_layers, stop_after, shapes)
    nc = _CACHE[key]
    res = run_bass_kernel_spmd(nc, in_maps, core_ids=list(range(NCORES)), trace=trace)
    out = np.concatenate([res.results[c]["out"] for c in range(NCORES)], axis=0)
    return out.reshape(1, SEQ, D).astype(np.float32), res


def kernel(**inputs):
    out, _ = run(inputs)
    return out
```
